# Optimizing a Trainium2 kernel written in Bass

```python
import jax, jax.numpy as jnp
from jax import lax
import numpy as np

D_MODEL = 1024
BATCH = 8
SEQ = 2048
DEPTH = 4

SB_HEADS = 8
SB_HEAD_DIM = 64
SB_WIDTH = SB_HEADS * SB_HEAD_DIM
SB_BLOCK = 128

GDN_HEADS = 4
GDN_HEAD_DIM = 128
GDN_WIDTH = GDN_HEADS * GDN_HEAD_DIM
GDN_CHUNK = 64
CONV_K = 4

EPS = 1e-6

SPLIT_SIZES = (3 * SB_WIDTH, SB_WIDTH, 3 * GDN_WIDTH, GDN_WIDTH, GDN_HEADS, GDN_HEADS, D_MODEL, D_MODEL)
N_IN = 3 * SB_WIDTH + SB_WIDTH + 3 * GDN_WIDTH + GDN_WIDTH + 2 * GDN_HEADS + 2 * D_MODEL

kernel_name = "stickbreak_gdn_gated_hybrid"


def rms_norm(x, w):
    xf = x.astype(jnp.float32)
    y = xf * lax.rsqrt(jnp.mean(xf * xf, axis=-1, keepdims=True) + EPS)
    return (y * w.astype(jnp.float32)).astype(x.dtype)


def l2_normalize(x):
    return x * lax.rsqrt(jnp.sum(x * x, axis=-1, keepdims=True) + EPS)


def split_heads(t, n_heads, head_dim):
    b, s, _ = t.shape
    return t.reshape(b, s, n_heads, head_dim).transpose(0, 2, 1, 3)


def merge_heads(t):
    b, h, s, d = t.shape
    return t.transpose(0, 2, 1, 3).reshape(b, s, h * d)


def stick_breaking_attention(q, k, v):
    seq = q.shape[2]
    scale = SB_HEAD_DIM ** -0.5
    outs = []
    for blk in range(seq // SB_BLOCK):
        t0 = blk * SB_BLOCK
        t1 = t0 + SB_BLOCK
        qb = q[:, :, t0:t1].astype(jnp.float32)
        kb = k[:, :, :t1].astype(jnp.float32)
        vb = v[:, :, :t1].astype(jnp.float32)
        z = jnp.einsum('bhqd,bhkd->bhqk', qb, kb) * scale
        q_pos = t0 + jnp.arange(SB_BLOCK)[:, None]
        k_pos = jnp.arange(t1)[None, :]
        causal = k_pos < q_pos
        log_keep = jnp.where(causal, -jax.nn.softplus(z), 0.0)
        suffix = lax.cumsum(log_keep, axis=3, reverse=True) - log_keep
        log_a = jax.nn.log_sigmoid(z) + suffix
        a = jnp.where(causal, jnp.exp(log_a), 0.0)
        outs.append(jnp.einsum('bhqk,bhkd->bhqd', a, vb))
    return jnp.concatenate(outs, axis=2).astype(q.dtype)


def causal_depthwise_conv(x, w):
    channels = x.shape[-1]
    return lax.conv_general_dilated(
        x, w[:, None, :].astype(x.dtype), window_strides=(1,), padding=[(CONV_K - 1, 0)],
        dimension_numbers=('NWC', 'WIO', 'NWC'), feature_group_count=channels)


def gated_delta_rule_chunked(q, k, v, g, beta):
    b, h, seq, dk = q.shape
    dv = v.shape[-1]
    c = GDN_CHUNK
    n = seq // c
    q = q * dk ** -0.5
    q = q.reshape(b, h, n, c, dk)
    k = k.reshape(b, h, n, c, dk)
    v = v.reshape(b, h, n, c, dv)
    beta = beta.reshape(b, h, n, c)
    g = jnp.cumsum(g.reshape(b, h, n, c), axis=-1)
    idx = jnp.arange(c)
    lower_incl = idx[:, None] >= idx[None, :]
    strict = idx[:, None] > idx[None, :]
    decay = jnp.exp(jnp.where(lower_incl, g[..., :, None] - g[..., None, :], -jnp.inf))
    kk = jnp.einsum('bhnid,bhnjd->bhnij', k, k)
    m = jnp.where(strict, beta[..., :, None] * kk * decay, 0.0)
    rhs = jnp.concatenate([v * beta[..., None], k * (beta * jnp.exp(g))[..., None]], axis=-1)
    sol = lax.linalg.triangular_solve(m, rhs, left_side=True, lower=True, unit_diagonal=True)
    u = sol[..., :dv]
    w = sol[..., dv:]
    intra = jnp.where(lower_incl, jnp.einsum('bhnid,bhnjd->bhnij', q, k) * decay, 0.0)

    def step(state, inp):
        q_c, k_c, u_c, w_c, g_c, intra_c = inp
        v_new = u_c - jnp.einsum('bhcd,bhde->bhce', w_c, state)
        o = (jnp.einsum('bhcd,bhde->bhce', q_c * jnp.exp(g_c)[..., None], state)
             + jnp.einsum('bhij,bhje->bhie', intra_c, v_new))
        g_last = g_c[..., -1:]
        state = (state * jnp.exp(g_last)[..., None]
                 + jnp.einsum('bhcd,bhce->bhde', k_c * jnp.exp(g_last - g_c)[..., None], v_new))
        return state, o

    chunk_first = lambda t: jnp.moveaxis(t, 2, 0)
    xs = tuple(chunk_first(t) for t in (q, k, u, w, g, intra))
    state0 = jnp.zeros((b, h, dk, dv), jnp.float32)
    _, o = lax.scan(step, state0, xs)
    return jnp.moveaxis(o, 0, 2).reshape(b, h, seq, dv)


def hybrid_layer(x, pre_w, post_w, w_in, conv_w, a_log, dt_bias, gdn_norm_w,
                 w_branch_sb, w_branch_gdn, w_out):
    f32 = jnp.float32
    h = rms_norm(x, pre_w)
    proj = h @ w_in
    points = [int(p) for p in np.cumsum(SPLIT_SIZES)[:-1]]
    sb_qkv, sb_gate, gdn_qkv, gdn_gate, gdn_a, gdn_b, merge_sb, merge_gdn = jnp.split(proj, points, axis=-1)

    q_sb, k_sb, v_sb = [split_heads(t, SB_HEADS, SB_HEAD_DIM) for t in jnp.split(sb_qkv, 3, axis=-1)]
    o_sb = merge_heads(stick_breaking_attention(q_sb, k_sb, v_sb)) * jax.nn.silu(sb_gate)

    gdn_qkv = jax.nn.silu(causal_depthwise_conv(gdn_qkv, conv_w))
    q_g, k_g, v_g = [split_heads(t, GDN_HEADS, GDN_HEAD_DIM).astype(f32) for t in jnp.split(gdn_qkv, 3, axis=-1)]
    q_g = l2_normalize(q_g)
    k_g = l2_normalize(k_g)
    g = (-jnp.exp(a_log.astype(f32)) * jax.nn.softplus(gdn_a.astype(f32) + dt_bias.astype(f32))).transpose(0, 2, 1)
    beta = jax.nn.sigmoid(gdn_b.astype(f32)).transpose(0, 2, 1)
    o_g = gated_delta_rule_chunked(q_g, k_g, v_g, g, beta)
    o_g = merge_heads(rms_norm(o_g, gdn_norm_w)).astype(x.dtype) * jax.nn.silu(gdn_gate)

    merged = (jax.nn.sigmoid(merge_sb) * (o_sb @ w_branch_sb)
              + jax.nn.sigmoid(merge_gdn) * (o_g @ w_branch_gdn))
    y = merged @ w_out
    return x + rms_norm(y, post_w)


def setup_inputs(seed: int = 0) -> dict:
    key = jax.random.key(seed)
    ks = jax.random.split(key, 12)
    f32 = jnp.float32

    def normal(k, shape, scale):
        return jax.random.normal(k, shape, f32) * scale

    x = normal(ks[0], (BATCH, SEQ, D_MODEL), 1.0)
    pre_norm_w = 1.0 + normal(ks[1], (DEPTH, D_MODEL), 0.05)
    post_norm_w = 1.0 + normal(ks[2], (DEPTH, D_MODEL), 0.05)
    w_in = normal(ks[3], (DEPTH, D_MODEL, N_IN), D_MODEL ** -0.5)
    conv_w = normal(ks[4], (DEPTH, CONV_K, 3 * GDN_WIDTH), CONV_K ** -0.5)
    a_log = jnp.log(jax.random.uniform(ks[5], (DEPTH, GDN_HEADS), f32, minval=0.5, maxval=16.0))
    dt = jnp.exp(jax.random.uniform(ks[6], (DEPTH, GDN_HEADS), f32,
                                    minval=math_log(0.001), maxval=math_log(0.1)))
    dt_bias = dt + jnp.log(-jnp.expm1(-dt))
    gdn_norm_w = 1.0 + normal(ks[7], (DEPTH, GDN_HEAD_DIM), 0.05)
    w_branch_sb = normal(ks[8], (DEPTH, SB_WIDTH, D_MODEL), SB_WIDTH ** -0.5)
    w_branch_gdn = normal(ks[9], (DEPTH, GDN_WIDTH, D_MODEL), GDN_WIDTH ** -0.5)
    w_out = normal(ks[10], (DEPTH, D_MODEL, D_MODEL), D_MODEL ** -0.5)
    return {"x": x, "pre_norm_w": pre_norm_w, "post_norm_w": post_norm_w, "w_in": w_in,
            "conv_w": conv_w, "a_log": a_log, "dt_bias": dt_bias, "gdn_norm_w": gdn_norm_w,
            "w_branch_sb": w_branch_sb, "w_branch_gdn": w_branch_gdn, "w_out": w_out}


def math_log(v):
    return float(np.log(v))


def reference(x, pre_norm_w, post_norm_w, w_in, conv_w, a_log, dt_bias, gdn_norm_w,
              w_branch_sb, w_branch_gdn, w_out):
    for layer in range(DEPTH):
        x = hybrid_layer(x, pre_norm_w[layer], post_norm_w[layer], w_in[layer], conv_w[layer],
                         a_log[layer], dt_bias[layer], gdn_norm_w[layer],
                         w_branch_sb[layer], w_branch_gdn[layer], w_out[layer])
    return x
```

```python
import contextlib
import numpy as np
import concourse.bass as bass
import concourse.mybir as mybir
from concourse.bass_utils import run_bass_kernel_spmd

F32 = mybir.dt.float32
BF16 = mybir.dt.bfloat16
AF = mybir.ActivationFunctionType
ALU = mybir.AluOpType

T = 2048
D = 1024
NIN = 6152
NCORES = 8
DEPTH = 4
EPS = 1e-6
NEG = -30000.0

ENGS = ["pe", "act", "dve", "pool", "sp"]


class Sched:
    NDMA = 24
    WIN = {"pe": 6, "act": 2, "dve": 2, "pool": 2, "sp": 0}

    def __init__(self, nc):
        self.nc = nc
        self.ops = {e: [] for e in ENGS}
        self.marks = {e: [] for e in ENGS}
        self.seenE = {e: {} for e in ENGS}
        self.seen = {e: {} for e in ENGS}
        self.lastw = {}
        self.readers = {}
        self.dma_n = [0] * self.NDMA
        self.dma_i = 0
        self.nsw = 0
        self.max_nsw = 0
        self.sw_gen = 0
        self.last_sw = None
        self.clear_ev = None
        self.out_events = []

    def _target(self, o, idx):
        import bisect
        m = self.marks[o]
        p = bisect.bisect_left(m, idx)
        if p < len(m) and m[p] - idx <= self.WIN[o]:
            return m[p]
        bisect.insort(m, idx)
        self.ops[o][idx]["mark"] = True
        return idx

    def _deps(self, eng, reads, writes):
        evs = []
        for k in reads:
            evs.extend(self.lastw.get(k, ()))
        for k in writes:
            evs.extend(self.lastw.get(k, ()))
            evs.extend(self.readers.get(k, ()))
        cur = len(self.ops[eng])
        needE = {}
        needD = {}
        for ev in evs:
            if ev[0] == "E":
                _, o, idx = ev
                if o == eng:
                    if eng == "pe" or cur - idx > 2:
                        continue
                if needE.get(o, -1) < idx:
                    needE[o] = idx
            else:
                sk, val = ev
                if needD.get(sk, 0) < val:
                    needD[sk] = val
        waits = []
        for o, idx in needE.items():
            if self.seenE[eng].get(o, -1) >= idx:
                continue
            j = self._target(o, idx)
            self.seenE[eng][o] = j
            waits.append(("E", o, j))
        for sk, val in needD.items():
            if self.seen[eng].get(sk, 0) >= val:
                continue
            self.seen[eng][sk] = val
            waits.append((sk, val))
        return waits

    def _record(self, ev, reads, writes):
        for k in writes:
            self.lastw[k] = [ev]
            self.readers[k] = []
        for k in reads:
            self.readers.setdefault(k, []).append(ev)

    def op(self, eng, fn, reads=(), writes=()):
        waits = self._deps(eng, reads, writes)
        idx = len(self.ops[eng])
        self.ops[eng].append({"fn": fn, "waits": waits, "mark": False, "inc": None})
        ev = ("E", eng, idx)
        self._record(ev, reads, writes)
        return ev

    def dma(self, eng, out, in_, reads=(), writes=(), is_output=False):
        s = self.dma_i % self.NDMA
        self.dma_i += 1
        sk = ("dma", s)
        waits = self._deps(eng, reads, writes)
        prev = 16 * self.dma_n[s]
        if prev > 0 and self.seen[eng].get(sk, 0) < prev:
            self.seen[eng][sk] = prev
            waits.append((sk, prev))
        self.dma_n[s] += 1
        ev = (sk, 16 * self.dma_n[s])
        self.ops[eng].append({"fn": (lambda e, o=out, i=in_: e.dma_start(out=o, in_=i)), "waits": waits, "mark": False,
                              "inc": (sk, 16)})
        self._record(ev, reads, writes)
        if is_output:
            self.out_events.append(ev)
        return ev

    def dma_sw(self, out, in_, reads=(), writes=()):
        waits = self._deps("pool", reads, writes)
        if self.last_sw is not None:
            psk = self.last_sw
            if self.seen["pool"].get(psk, 0) < 16:
                self.seen["pool"][psk] = 16
                waits.append((psk, 16))
        sk = ("sw", self.nsw, self.sw_gen)
        self.nsw += 1
        self.max_nsw = max(self.max_nsw, self.nsw)
        self.last_sw = sk
        ev = (sk, 16)
        self.ops["pool"].append({"fn": (lambda e, o=out, i=in_: e.dma_start(out=o, in_=i)), "waits": waits, "mark": False,
                                 "inc": (sk, 16)})
        self._record(ev, reads, writes)
        if self.clear_ev is not None:
            for k in writes:
                self.lastw[k] = [ev, self.clear_ev]
        return ev

    def sw_reset(self):
        n = self.nsw
        if n == 0:
            return
        for i in range(n):
            self.ops["pool"].append({"fn": (lambda e, i=i: e.sem_clear(self.sems[("sw", i)])), "waits": [], "mark": False,
                                     "inc": None})
        idx = len(self.ops["pool"]) - 1
        j = self._target("pool", idx)
        self.clear_ev = ("E", "pool", j)
        self.nsw = 0
        self.sw_gen += 1
        self.last_sw = None

    def _last_compute(self, o):
        for i in range(len(self.ops[o]) - 1, -1, -1):
            d = self.ops[o][i]
            if d["fn"] is not None and d["inc"] is None:
                return i
        return -1

    def barrier(self):
        last = {o: self._last_compute(o) for o in ENGS}
        for e in ENGS:
            waits = []
            for o in ENGS:
                if o == e or last[o] < 0:
                    continue
                if self.seenE[e].get(o, -1) >= last[o]:
                    continue
                j = self._target(o, last[o])
                self.seenE[e][o] = j
                waits.append(("E", o, j))
            for s in range(self.NDMA):
                v = 16 * self.dma_n[s]
                sk = ("dma", s)
                if v > 0 and self.seen[e].get(sk, 0) < v:
                    self.seen[e][sk] = v
                    waits.append((sk, v))
            for s in range(self.nsw):
                sk = ("sw", s, self.sw_gen)
                if self.seen[e].get(sk, 0) < 16:
                    self.seen[e][sk] = 16
                    waits.append((sk, 16))
            if waits:
                self.ops[e].append({"fn": None, "waits": waits, "mark": False, "inc": None})

    def finish(self):
        need = {}
        for sk, val in self.out_events:
            need[sk] = max(need.get(sk, 0), val)
        self.ops["sp"].append({"fn": None, "waits": list(need.items()), "mark": False, "inc": None})

    def emit(self):
        nc = self.nc
        count_at = {}
        for e in ENGS:
            c = 0
            arr = []
            for d in self.ops[e]:
                if d["mark"]:
                    c += 1
                arr.append(c)
            count_at[e] = arr
        self.n_inc = {e: (count_at[e][-1] if count_at[e] else 0) for e in ENGS}
        with contextlib.ExitStack() as st:
            sems = {}
            for e in ENGS:
                sems[e] = st.enter_context(nc.semaphore("s_" + e))
            for i in range(self.NDMA):
                sems[("dma", i)] = st.enter_context(nc.semaphore("s_dma%d" % i))
            for i in range(self.max_nsw):
                sems[("sw", i)] = st.enter_context(nc.semaphore("s_sw%d" % i))
            self.sems = sems
            block = st.enter_context(nc.Block())

            def semof(sk):
                if isinstance(sk, tuple) and sk[0] == "sw":
                    return sems[("sw", sk[1])]
                return sems[sk]

            def run(engobj, ename):
                for d in self.ops[ename]:
                    for w in d["waits"]:
                        if w[0] == "E":
                            engobj.wait_ge(sems[w[1]], count_at[w[1]][w[2]])
                        else:
                            engobj.wait_ge(semof(w[0]), w[1])
                    if d["fn"] is None:
                        continue
                    ins = d["fn"](engobj)
                    if d["inc"] is not None:
                        ins.then_inc(semof(d["inc"][0]), d["inc"][1])
                    elif d["mark"]:
                        ins.then_inc(sems[ename], 1)

            @block.tensor
            def _(e):
                run(e, "pe")

            @block.scalar
            def _(e):
                run(e, "act")

            @block.vector
            def _(e):
                run(e, "dve")

            @block.gpsimd
            def _(e):
                run(e, "pool")

            @block.sync
            def _(e):
                run(e, "sp")


class _Stop(Exception):
    pass


def build_program(NL, dbg=False, stop=99, part=0):
    nc = bass.Bass("TRN2", target_bir_lowering=False)
    dt_in = lambda n, s: nc.dram_tensor(n, s, F32, kind="ExternalInput").ap()
    x_d = dt_in("x", [T, D])
    prew_d = dt_in("pre_w", [NL, D])
    postw_d = dt_in("post_w", [NL, D])
    win_d = dt_in("w_in", [NL, D, NIN])
    convw_d = dt_in("conv_w", [NL, 128, 48])
    alog_d = dt_in("a_log", [NL, 64])
    dtb_d = dt_in("dt_bias", [NL, 64])
    gnw_d = dt_in("gdn_norm_w", [NL, 128])
    wbs_d = dt_in("w_branch_sb", [NL, 512, D])
    wbg_d = dt_in("w_branch_gdn", [NL, 512, D])
    wout_d = dt_in("w_out", [NL, D, D])
    out_d = nc.dram_tensor("out", [T, D], F32, kind="ExternalOutput").ap()
    osb_out = nc.dram_tensor("osb_o", [512, T], F32, kind="ExternalOutput").ap() if part == 1 else None
    osb_in = dt_in("osb_in", [512, T]) if part == 2 else None
    dbg_d = {}
    if dbg:
        for n, s in [("d_osb", [512, T]), ("d_ogt", [512, T]), ("d_q", [512, T]), ("d_gq", [512, T]),
                     ("d_gk", [512, T]), ("d_gv", [512, T]), ("d_cols", [128, 7 * 64]), ("d_mT", [1024, T])]:
            dbg_d[n] = nc.dram_tensor(n, s, F32, kind="ExternalOutput").ap()

    S = Sched(nc)
    with contextlib.ExitStack() as st:
        def sb(n, s, d):
            return st.enter_context(nc.sbuf_tensor(n, s, d))

        def psum(n, s, d):
            return st.enter_context(nc.psum_tensor(n, s, d))

        def mm(out, lhsT, rhs, start=True, stop=True, r=(), w=()):
            return S.op("pe", lambda e: e.matmul(out, lhsT=lhsT, rhs=rhs, start=start, stop=stop), r, w)

        def tr(out, in_, idn, r=(), w=()):
            return S.op("pe", lambda e: e.transpose(out=out, in_=in_, identity=idn), r, w)

        def act(out, in_, func, r=(), w=(), bias=0.0, scale=1.0, accum=None):
            if accum is None:
                return S.op("act", lambda e: e.activation(out=out, in_=in_, func=func, bias=bias, scale=scale), r, w)
            return S.op("act", lambda e: e.activation(out=out, in_=in_, func=func, bias=bias, scale=scale, accum_out=accum), r, w)

        def tt(eng, out, in0, in1, op, r=(), w=()):
            return S.op(eng, lambda e: e.tensor_tensor(out=out, in0=in0, in1=in1, op=op), r, w)

        def ts(eng, out, in0, s1, op0, r=(), w=()):
            return S.op(eng, lambda e: e.tensor_scalar(out=out, in0=in0, scalar1=s1, scalar2=None, op0=op0), r, w)

        def stt(eng, out, in0, scalar, in1, op0, op1, r=(), w=()):
            return S.op(eng, lambda e: e.scalar_tensor_tensor(out=out, in0=in0, scalar=scalar, in1=in1, op0=op0, op1=op1), r, w)

        def cp(eng, out, in_, r=(), w=()):
            return S.op(eng, lambda e: e.tensor_copy(out=out, in_=in_), r, w)

        def memset(eng, ap, val, w=()):
            return S.op(eng, lambda e: e.memset(ap, val), (), w)

        def asel(out, in_, pattern, cmp, fill, base, cm, r=(), w=()):
            return S.op("pool", lambda e: e.affine_select(out=out, in_=in_, pattern=pattern, compare_op=cmp, fill=fill,
                                                           base=base, channel_multiplier=cm), r, w)

        identf = sb("identf", [128, 128], F32)
        ident = sb("ident", [128, 128], BF16)
        tria = sb("tria", [128, 128], BF16)
        negcol = sb("negcol", [128, 1], BF16)
        ones1 = sb("ones1", [1, 128], BF16)
        maskneg = sb("maskneg", [128, 896], BF16)
        masks2 = sb("masks2", [128, 256], F32)
        triincl = sb("triincl", [128, 128], F32)
        onesf = sb("onesf", [128, 128], F32)
        onesb = sb("onesb", [128, 128], BF16)
        ARENA = sb("arena", [128, 10240], F32)
        AR = ARENA[:]

        def af(a, b):
            return AR[:, a:b]

        def ab16(a, b):
            return AR[:, a:b].bitcast(BF16)

        tmpc = af(0, 896)
        memset("pool", identf[:], 0.0, w=["identf"])
        asel(identf[:], identf[:], [[1, 128]], ALU.not_equal, 1.0, 0, -1, r=["identf"], w=["identf"])
        cp("dve", ident[:], identf[:], r=["identf"], w=["ident"])
        memset("pool", onesf[:], 1.0, w=["onesf"])
        cp("dve", onesb[:], onesf[:], r=["onesf"], w=["onesb"])
        cp("dve", ones1[:], onesf[0:1, :], r=["onesf"], w=["ones1"])
        memset("dve", negcol[:], -1.0, w=["negcol"])
        memset("pool", tmpc[:, 0:128], -1.0, w=["tmpc"])
        asel(tmpc[:, 0:128], tmpc[:, 0:128], [[-1, 128]], ALU.is_ge, 0.0, 0, 1, r=["tmpc"], w=["tmpc"])
        cp("dve", tria[:], tmpc[:, 0:128], r=["tmpc"], w=["tria"])
        asel(triincl[:], onesf[:], [[1, 128]], ALU.is_ge, 0.0, 0, -1, r=["onesf"], w=["triincl"])
        memset("pool", tmpc, 0.0, w=["tmpc"])
        asel(tmpc, tmpc, [[1, 896]], ALU.is_gt, NEG, -384, -1, r=["tmpc"], w=["tmpc"])
        cp("dve", maskneg[:], tmpc, r=["tmpc"], w=["maskneg"])
        memset("pool", masks2[:], 0.0, w=["masks2"])
        asel(masks2[:, 0:128], masks2[:, 0:128], [[1, 128]], ALU.is_ge, NEG, 0, -1, r=["masks2"], w=["masks2"])
        asel(masks2[:, 128:256], masks2[:, 128:256], [[1, 128]], ALU.is_gt, NEG, 0, -1, r=["masks2"], w=["masks2"])
        S.barrier()

        hT = sb("hT", [128, 8, T], BF16)
        BIG = [sb("big%d" % i, [128, 4, T], BF16) for i in range(4)]
        OSB = sb("osb", [128, 4, T], BF16)
        OGT = sb("ogt", [128, 4, T], BF16)
        NW = 3
        WB = [sb("wb%d" % i, [128, 4096], BF16) for i in range(NW)]
        PS = [psum("ps%d" % i, [128, 512], F32) for i in range(8)]
        ssc = sb("ssc", [128, 8], F32)
        convw = sb("convw", [128, 48], F32)
        gnw = sb("gnw", [128, 1], F32)
        alogb = sb("alogb", [128, 64], F32)
        dtbb = sb("dtbb", [128, 64], F32)
        wab = sb("wab", [128, 8, 8], BF16)
        abc = sb("abc", [128, 128], F32)
        colnames = ["graw", "lnb", "gcum", "negg", "ebg", "edec", "beta", "eglast", "tmpa", "tmpb"]
        COL = {n: sb("col_" + n, [128, 64], F32) for n in colnames}
        Sst = [sb("S%d" % h, [128, 128], F32) for h in range(4)]
        Sbf = [sb("Sb%d" % h, [128, 128], BF16) for h in range(4)]

        T512 = [af(0, 512), af(512, 1024)]
        xt = [af(1024, 2048), af(2048, 3072)]
        junk = ab16(3072, 3584)
        xn = [ab16(3584, 4096), ab16(4096, 4608)]
        ppwb = af(4608, 5632)
        EB = [af(i * 512, (i + 1) * 512) for i in range(3)]
        LB = [ab16(1536 + i * 256, 1536 + (i + 1) * 256) for i in range(4)]
        AB = [ab16(2560 + i * 256, 2560 + (i + 1) * 256) for i in range(3)]
        RH = [AR[0:1, 3328 + i * 512:3328 + (i + 1) * 512].bitcast(BF16) for i in range(4)]
        RACC = [AR[0:1, 5376 + i * 512:5376 + (i + 1) * 512] for i in range(2)]
        CV = af(0, T + 4)
        ACC = af(2052, 2052 + T)
        SQ = ab16(4100, 5124)
        T512c = [af(5124, 5636), af(5636, 6148)]
        def hv(h, off, n, bf=False):
            base = h * 2496 + off
            return ab16(base, base + n) if bf else af(base, base + n)
        GB2 = [hv(h, 0, 256) for h in range(4)]
        GM2 = [hv(h, 256, 256) for h in range(4)]
        ET2 = [hv(h, 512, 256) for h in range(4)]
        EG = [hv(h, 768, 128) for h in range(4)]
        PP = [[hv(h, 896, 256), hv(h, 1152, 256)] for h in range(4)]
        XX = [hv(h, 1408, 128) for h in range(4)]
        OTC = [hv(h, 1536, 128) for h in range(4)]
        R12 = [hv(h, 1664, 128) for h in range(4)]
        OG1 = [hv(h, 1792, 128) for h in range(4)]
        bfn = ["INTRA", "QG", "XB", "KBG", "KDEC", "VB", "WTN", "VN", "OSQ"]
        BFT = {nm: [hv(h, 1920 + 64 * j, 64, bf=True) for h in range(4)] for j, nm in enumerate(bfn)}
        INTRA, QG, XB, KBG, KDEC, VBt, WTN, VN, OSQ = [BFT[nm] for nm in bfn]

        wlist = []
        for l in range(NL):
            if part in (0, 1):
                for c0 in (0, 512, 1024, 1536):
                    wlist.append(("in", l, c0))
            if part in (0, 2):
                for c0 in (2048, 2560, 3072, 3584, 3592):
                    wlist.append(("in", l, c0))
                wlist += [("in", l, 4104), ("wbs", l, 0), ("in", l, 4616), ("in", l, 5128), ("wbg", l, 0), ("in", l, 5640),
                          ("wout", l, 0), ("wout", l, 512)]
        wst = {"issued": 0, "next": 0}

        def w_issue(k):
            kind, l_, c0 = wlist[k]
            wi = k % NW
            if kind in ("in", "wout"):
                src_t = win_d if kind == "in" else wout_d
                view = WB[wi][:].rearrange("p (k c) -> p k c", k=8)
                src = src_t[l_][:, c0:c0 + 512].rearrange("(k p) n -> p k n", p=128)
            else:
                src_t = wbs_d if kind == "wbs" else wbg_d
                view = WB[wi][:].rearrange("p (k c) -> p k c", k=4)
                src = src_t[l_].rearrange("(k p) n -> p k n", p=128)
            S.dma_sw(view, src, writes=[("wb", wi)])

        def wget(keep=0):
            k = wst["next"]
            wst["next"] += 1
            while wst["issued"] < min(len(wlist), k - keep + NW):
                w_issue(wst["issued"])
                wst["issued"] += 1
            return k % NW

        psi = {"i": 0}

        def nextps():
            i = psi["i"] % 8
            psi["i"] += 1
            return i

        def cut(k):
            if stop <= k:
                raise _Stop()

        for l in range(NL):
          try:
              xin = x_d if l == 0 else out_d
              xkey = (lambda t_: ("xin0", t_)) if l == 0 else (lambda t_: ("out", t_))
              S.dma("sp", ppwb, prew_d[l].partition_broadcast(128), writes=["ppwb"])
              S.dma("sp", convw[:], convw_d[l], writes=["convw"])
              S.dma("sp", gnw[:], gnw_d[l].rearrange("(p o) -> p o", o=1), writes=["gnw"])
              S.dma("sp", alogb[:], alog_d[l].partition_broadcast(128), writes=["alogb"])
              S.dma("sp", dtbb[:], dtb_d[l].partition_broadcast(128), writes=["dtbb"])

              for t_ in range(16):
                  b = t_ % 2
                  S.dma("sp", xt[b], xin[t_ * 128:(t_ + 1) * 128, :], reads=[xkey(t_)], writes=[("xt", b)])
                  act(junk, xt[b], AF.Square, r=[("xt", b)], w=["junk", ("ssc", b)], accum=ssc[:, b:b + 1])
                  act(ssc[:, 2 + b:3 + b], ssc[:, b:b + 1], AF.Ln, r=[("ssc", b)], w=[("ssc2", b)], scale=1.0 / D, bias=EPS)
                  act(ssc[:, 4 + b:5 + b], ssc[:, 2 + b:3 + b], AF.Exp, r=[("ssc2", b)], w=[("ssc4", b)], scale=-0.5)
                  stt("dve", xn[b], xt[b], ssc[:, 4 + b:5 + b], ppwb, ALU.mult, ALU.mult,
                      r=[("xt", b), ("ssc4", b), "ppwb"], w=[("xn", b)])
                  pi = nextps()
                  pbv = PS[pi][:].bitcast(BF16)
                  for kc in range(8):
                      tr(pbv[:, kc * 128:(kc + 1) * 128], xn[b][:, kc * 128:(kc + 1) * 128], ident[:],
                         r=[("xn", b), "ident"], w=[("ps", pi)])
                  if t_ % 2 == 0:
                      cp("dve", hT[:, :, t_ * 128:(t_ + 1) * 128], pbv.rearrange("p (k c) -> p k c", k=8),
                         r=[("ps", pi)], w=["hT"])
                  else:
                      act(hT[:, :, t_ * 128:(t_ + 1) * 128], pbv.rearrange("p (k c) -> p k c", k=8), AF.Copy,
                          r=[("ps", pi)], w=["hT"])

              cut(1)
              if part != 2:
               if True:
                  QT, KT, VV, GT = BIG
                  Vtok = VV[:].rearrange("p a (b c) -> p (a b) c", c=512)

                  def proj_fm(wi, evac):
                      wv = WB[wi][:].rearrange("p (k c) -> p k c", k=8)
                      for fc in range(4):
                          for tg in range(4):
                              pi = nextps()
                              for kc in range(8):
                                  mm(PS[pi][:], wv[:, kc, fc * 128:(fc + 1) * 128], hT[:, kc, tg * 512:(tg + 1) * 512],
                                     start=(kc == 0), stop=(kc == 7), r=[("wb", wi), "hT"], w=[("ps", pi)])
                              evac(pi, fc, tg)

                  wi = wget()
                  proj_fm(wi, lambda pi, fc, tg: act(QT[:, fc, tg * 512:(tg + 1) * 512], PS[pi][:], AF.Copy,
                                                     r=[("ps", pi)], w=[("big", 0, fc)], scale=0.125))
                  wi = wget()
                  proj_fm(wi, lambda pi, fc, tg: cp("dve", KT[:, fc, tg * 512:(tg + 1) * 512], PS[pi][:],
                                                    r=[("ps", pi)], w=[("big", 1, fc)]))
                  wi = wget()
                  wv = WB[wi][:].rearrange("p (k c) -> p k c", k=8)
                  for t_ in range(16):
                      pi = nextps()
                      for kc in range(8):
                          mm(PS[pi][:], hT[:, kc, t_ * 128:(t_ + 1) * 128], wv[:, kc, :], start=(kc == 0), stop=(kc == 7),
                             r=[("wb", wi), "hT"], w=[("ps", pi)])
                      if t_ % 2 == 0:
                          cp("dve", Vtok[:, t_, :], PS[pi][:], r=[("ps", pi)], w=[("big", 2, t_ // 4)])
                      else:
                          act(Vtok[:, t_, :], PS[pi][:], AF.Copy, r=[("ps", pi)], w=[("big", 2, t_ // 4)])
                  wi = wget()
                  proj_fm(wi, lambda pi, fc, tg: act(GT[:, fc, tg * 512:(tg + 1) * 512], PS[pi][:], AF.Silu,
                                                     r=[("ps", pi)], w=[("big", 3, fc)]))

                  cut(2)
                  S.barrier()
                  tiles = []
                  for fc in range(4):
                      for G in range(4):
                          for kb in range(4 * G + 3, -1, -1):
                              for hh in range(2):
                                  tiles.append((fc, G, kb, hh))
                  nt = len(tiles)
                  info = {}

                  def st1(i):
                      fc, G, kb, hh = tiles[i]
                      p0 = 64 * hh
                      top = 4 * G + 3
                      diag = kb >= 4 * G
                      kbl = kb - 4 * G
                      zb = i % 2
                      eb = i % 3
                      lb = i % 4
                      kT_ = KT[p0:p0 + 64, fc, kb * 128:(kb + 1) * 128]
                      qT_ = QT[p0:p0 + 64, fc, G * 512:(G + 1) * 512]
                      mk_ = maskneg[:, 384 - 128 * kbl:384 - 128 * kbl + 512] if diag else None
                      mm(PS[zb][:], kT_, qT_, start=True, stop=not diag, r=[("big", 1, fc), ("big", 0, fc)], w=[("ps", zb)])
                      if diag:
                          mm(PS[zb][:], ident[:], mk_, start=False, stop=True, r=["ident", "maskneg"], w=[("ps", zb)])
                      act(EB[eb], PS[zb][:], AF.Exp, r=[("ps", zb)], w=[("eb", eb)])
                      act(LB[lb], EB[eb], AF.Ln, r=[("eb", eb)], w=[("lb", lb)], bias=1.0)
                      info[i] = (kT_, qT_, mk_, diag, top, lb)

                  def st2(i):
                      fc, G, kb, hh = tiles[i]
                      kT_, qT_, mk_, diag, top, lb = info[i]
                      la = 2 + (i % 2)
                      rb = 4 + hh
                      ab = i % 3
                      rh = RH[2 * hh + (kb % 2)]
                      rhp = RH[2 * hh + ((kb + 1) % 2)]
                      mm(PS[la][:], kT_, qT_, start=True, stop=False, r=[("big", 1, fc), ("big", 0, fc)], w=[("ps", la)])
                      if diag:
                          mm(PS[la][:], ident[:], mk_, start=False, stop=False, r=["ident", "maskneg"], w=[("ps", la)])
                      last = (kb == top)
                      mm(PS[la][:], tria[:], LB[lb], start=False, stop=last, r=["tria", ("lb", lb)], w=[("ps", la)])
                      if not last:
                          mm(PS[la][:], ones1[:], rhp[:, 0:512], start=False, stop=False, r=["ones1", ("rh", hh, (kb + 1) % 2)], w=[("ps", la)])
                          mm(PS[la][:], ones1[:], rhp[:, 512:1024], start=False, stop=True, r=["ones1", ("rh", hh, (kb + 1) % 2)], w=[("ps", la)])
                      if kb > 0:
                          mm(PS[rb][0:1, :], negcol[:], LB[lb], start=True, stop=True, r=["negcol", ("lb", lb)], w=[("ps", rb)])
                          if last:
                              cp("dve", RACC[hh], PS[rb][0:1, :], r=[("ps", rb)], w=[("racc", hh)])
                          else:
                              tt("dve", RACC[hh], RACC[hh], PS[rb][0:1, :], ALU.add, r=[("ps", rb), ("racc", hh)], w=[("racc", hh)])
                          cp("dve", rh[:, 0:512], RACC[hh], r=[("racc", hh)], w=[("rh", hh, kb % 2)])
                          tt("dve", rh[:, 512:1024], RACC[hh], rh[:, 0:512], ALU.subtract, r=[("racc", hh), ("rh", hh, kb % 2)],
                             w=[("rh", hh, kb % 2)])
                      act(AB[ab], PS[la][:], AF.Exp, r=[("ps", la)], w=[("ab", ab)])

                  def st3(i):
                      fc, G, kb, hh = tiles[i]
                      p0 = 64 * hh
                      top = 4 * G + 3
                      ab = i % 3
                      ob = 6 + hh
                      vv = Vtok[:, kb, (2 * fc + hh) * 64:(2 * fc + hh + 1) * 64]
                      mm(PS[ob][p0:p0 + 64, :], vv, AB[ab], start=(kb == top), stop=(kb == 0),
                         r=[("big", 2, kb // 4), ("ab", ab)], w=[("ps", ob)])
                      if kb == 0:
                          tt("dve", OSB[p0:p0 + 64, fc, G * 512:(G + 1) * 512], PS[ob][p0:p0 + 64, :],
                             GT[p0:p0 + 64, fc, G * 512:(G + 1) * 512], ALU.mult, r=[("ps", ob), ("big", 3, fc)], w=[("osb", fc)])

                  for i in range(nt + 3):
                      if i < nt:
                          st1(i)
                      if 0 <= i - 1 < nt:
                          st2(i - 1)
                      if 0 <= i - 2 < nt:
                          st3(i - 2)
                  S.barrier()

                  if dbg and l == 0:
                      for fc in range(4):
                          for half in range(2):
                              sl = slice(half * 1024, (half + 1) * 1024)
                              cp("dve", ACC[:, 0:1024], OSB[:, fc, sl], r=[("osb", fc)], w=["acc"])
                              S.dma("sp", dbg_d["d_osb"][fc * 128:(fc + 1) * 128, sl], ACC[:, 0:1024], reads=["acc"], is_output=True)
                              cp("dve", ACC[:, 0:1024], QT[:, fc, sl], r=[("big", 0, fc)], w=["acc"])
                              S.dma("sp", dbg_d["d_q"][fc * 128:(fc + 1) * 128, sl], ACC[:, 0:1024], reads=["acc"], is_output=True)
                      S.barrier()

              if part == 1:
                  for fc in range(4):
                      for half in range(2):
                          sl = slice(half * 1024, (half + 1) * 1024)
                          hb = af(2052 + half * 1024, 2052 + (half + 1) * 1024)
                          cp("dve", hb, OSB[:, fc, sl], r=[("osb", fc)], w=[("accd", half)])
                          S.dma("sp", osb_out[fc * 128:(fc + 1) * 128, sl], hb, reads=[("accd", half)], is_output=True)
                  raise _Stop()
              if part == 2:
                  for fc in range(4):
                      for half in range(2):
                          sl = slice(half * 1024, (half + 1) * 1024)
                          S.dma_sw(OSB[:, fc, sl], osb_in[fc * 128:(fc + 1) * 128, sl], writes=[("osb", fc, half)])
                  S.barrier()
              cut(3)
              GQ, GK, GV, GG = BIG
              memset("dve", CV[:, 0:4], 0.0, w=["cv"])
              for grp in range(4):
                  wi = wget()
                  wv = WB[wi][:].rearrange("p (k c) -> p k c", k=8)
                  dst = BIG[grp]
                  for h in range(4):
                      for tg in range(4):
                          pi = nextps()
                          for kc in range(8):
                              mm(PS[pi][:], wv[:, kc, h * 128:(h + 1) * 128], hT[:, kc, tg * 512:(tg + 1) * 512],
                                 start=(kc == 0), stop=(kc == 7), r=[("wb", wi), "hT"], w=[("ps", pi)])
                          if grp == 3:
                              act(dst[:, h, tg * 512:(tg + 1) * 512], PS[pi][:], AF.Silu, r=[("ps", pi)], w=[("big", 3, h)])
                          else:
                              act(CV[:, 4 + tg * 512:4 + (tg + 1) * 512], PS[pi][:], AF.Copy, r=[("ps", pi)], w=["cv"])
                      if grp == 3:
                          continue
                      ch = grp * 4 + h
                      ts("dve", ACC, CV[:, 4:4 + T], convw[:, ch * 4 + 3:ch * 4 + 4], ALU.mult, r=["cv", "convw"], w=["acc"])
                      for i_ in range(3):
                          stt("dve", ACC, CV[:, 1 + i_:1 + i_ + T], convw[:, ch * 4 + i_:ch * 4 + i_ + 1], ACC, ALU.mult, ALU.add,
                              r=["cv", "convw", "acc"], w=["acc"])
                      if grp == 2:
                          act(dst[:, h, :], ACC, AF.Silu, r=["acc"], w=[("big", 2, h)])
                          continue
                      act(ACC, ACC, AF.Silu, r=["acc"], w=["acc"])
                      act(SQ, ACC, AF.Square, r=["acc"], w=["sq"])
                      for tg in range(4):
                          pi = nextps()
                          mm(PS[pi][:], onesb[:], SQ[:, tg * 512:(tg + 1) * 512], r=["onesb", "sq"], w=[("ps", pi)])
                          tb = tg % 2
                          act(T512c[tb], PS[pi][:], AF.Ln, r=[("ps", pi)], w=[("t512c", tb)], bias=EPS)
                          act(T512c[tb], T512c[tb], AF.Exp, r=[("t512c", tb)], w=[("t512c", tb)], scale=-0.5,
                              bias=(-0.5 * float(np.log(128.0)) if grp == 0 else 0.0))
                          tt("dve", dst[:, h, tg * 512:(tg + 1) * 512], ACC[:, tg * 512:(tg + 1) * 512], T512c[tb], ALU.mult,
                             r=["acc", ("t512c", tb)], w=[("big", grp, h)])

              if dbg and l == 0:
                  for nm, bi in (("d_gq", 0), ("d_gk", 1), ("d_gv", 2)):
                      for h in range(4):
                          cp("dve", ACC, BIG[bi][:, h, :], r=[("big", bi, h)], w=["acc"])
                          S.dma("sp", dbg_d[nm][h * 128:(h + 1) * 128, :], ACC, reads=["acc"], is_output=True)

              cut(4)
              wi = wget()
              wv = WB[wi][:].rearrange("p (k c) -> p k c", k=8)
              for t_ in range(16):
                  pi = nextps()
                  for kc in range(8):
                      mm(PS[pi][:], hT[:, kc, t_ * 128:(t_ + 1) * 128], wv[:, kc, :], start=(kc == 0), stop=(kc == 7),
                         r=[("wb", wi), "hT"], w=[("ps", pi)])
                  cp("dve", abc[:, t_ * 8:(t_ + 1) * 8], PS[pi][:, 504:512], r=[("ps", pi)], w=["abc"])
              cut(4.1)
              abv = abc[:].rearrange("p (t c) -> p t c", c=8)
              c3 = lambda n: COL[n][:].rearrange("p (t c) -> p t c", c=4)
              tt("dve", c3("tmpa"), abv[:, :, 0:4], dtbb[:].rearrange("p (t c) -> p t c", c=4), ALU.add, r=["abc", "dtbb"], w=["c_tmpa"])
              act(COL["tmpa"][:], COL["tmpa"][:], AF.Exp, r=["c_tmpa"], w=["c_tmpa"])
              act(COL["tmpa"][:], COL["tmpa"][:], AF.Ln, r=["c_tmpa"], w=["c_tmpa"], bias=1.0)
              act(COL["tmpb"][:], alogb[:], AF.Exp, r=["alogb"], w=["c_tmpb"])
              stt("dve", COL["graw"][:], COL["tmpa"][:], -1.0, COL["tmpb"][:], ALU.mult, ALU.mult, r=["c_tmpa", "c_tmpb"], w=["c_graw"])
              cut(4.2)
              act(c3("tmpa"), abv[:, :, 4:8], AF.Exp, r=["abc", "c_graw"], w=["c_tmpa"], scale=-1.0)
              act(COL["tmpa"][:], COL["tmpa"][:], AF.Ln, r=["c_tmpa"], w=["c_tmpa"], bias=1.0)
              ts("dve", COL["lnb"][:], COL["tmpa"][:], -1.0, ALU.mult, r=["c_tmpa"], w=["c_lnb"])
              cut(4.3)
              pi = nextps()
              mm(PS[pi][:, 0:64], triincl[:], COL["graw"][:], r=["triincl", "c_graw"], w=[("ps", pi)])
              mm(PS[pi][:, 64:128], onesf[:], COL["graw"][:], r=["onesf", "c_graw"], w=[("ps", pi)])
              cp("dve", COL["gcum"][:], PS[pi][:, 0:64], r=[("ps", pi)], w=["c_gcum"])
              ts("dve", COL["negg"][:], PS[pi][:, 0:64], -1.0, ALU.mult, r=[("ps", pi)], w=["c_negg"])
              cut(4.4)
              tt("dve", COL["tmpb"][:], COL["gcum"][:], COL["lnb"][:], ALU.add, r=["c_gcum", "c_lnb", "c_graw"], w=["c_tmpb"])
              act(COL["ebg"][:], COL["tmpb"][:], AF.Exp, r=["c_tmpb"], w=["c_ebg"])
              tt("dve", COL["tmpa"][:], PS[pi][:, 64:128], COL["gcum"][:], ALU.subtract, r=[("ps", pi), "c_gcum", "c_lnb"], w=["c_tmpa"])
              act(COL["edec"][:], COL["tmpa"][:], AF.Exp, r=["c_tmpa"], w=["c_edec"])
              act(COL["beta"][:], COL["lnb"][:], AF.Exp, r=["c_lnb"], w=["c_beta"])
              act(COL["eglast"][:], PS[pi][:, 64:128], AF.Exp, r=[("ps", pi)], w=["c_eglast"])
              cut(4.5)
              if dbg and l == 0:
                  for j, nm in enumerate(["graw", "lnb", "gcum", "negg", "ebg", "edec", "beta"]):
                      S.dma("sp", dbg_d["d_cols"][:, j * 64:(j + 1) * 64], COL[nm][:], reads=["c_" + nm, "c_tmpb"], is_output=True)
              S.barrier()

              cut(5)
              import os as _os
              for n in range(int(_os.environ.get('GDN_NMIN', '0')), int(_os.environ.get('GDN_NMAX', '16'))):
                  c0 = n * 128
                  hs = range(4)
                  for h in hs:
                      idx = n * 4 + h
                      kA = ("ps", n % 2)
                      A_ = PS[n % 2]
                      kTc = GK[:, h, c0:c0 + 128]
                      qTc = GQ[:, h, c0:c0 + 128]
                      cp("dve", GB2[h][:, 0:128], COL["gcum"][:, idx:idx + 1].to_broadcast([128, 128]), r=["c_gcum"], w=[("gb2", h)])
                      cp("dve", GB2[h][:, 128:256], COL["tmpb"][:, idx:idx + 1].to_broadcast([128, 128]), r=["c_tmpb"], w=[("gb2", h)])
                      mm(A_[:, 0:128], GB2[h][:, 0:128], identf[:], r=[("gb2", h), "identf"], w=[kA])
                      mm(A_[:, 128:256], GB2[h][:, 128:256], identf[:], r=[("gb2", h), "identf"], w=[kA])
                      mm(A_[:, 256:384], kTc, kTc, r=[("big", 1, h)], w=[kA])
                      mm(A_[:, 384:512], kTc, qTc, r=[("big", 1, h), ("big", 0, h)], w=[kA])
                      tt("dve", GM2[h], A_[:, 0:256], masks2[:], ALU.add, r=[kA, "masks2"], w=[("gm2", h)])
                      act(ET2[h], GM2[h], AF.Exp, r=[("gm2", h), "c_negg"], w=[("et2", h)], bias=COL["negg"][:, idx:idx + 1])
                      act(EG[h], A_[:, 0:128], AF.Exp, r=[kA], w=[("eg", h)])
                      tt("dve", INTRA[h], A_[:, 384:512], ET2[h][:, 0:128], ALU.mult, r=[kA, ("et2", h)], w=[("intra", h)])
                      stt("dve", PP[h][0][:, 0:128], A_[:, 256:384], -1.0, ET2[h][:, 128:256], ALU.mult, ALU.mult,
                          r=[kA, ("et2", h)], w=[("pp", h, 0)])
                      tt("dve", QG[h], qTc, EG[h], ALU.mult, r=[("big", 0, h), ("eg", h)], w=[("qg", h)])
                      tt("dve", XX[h], PP[h][0][:, 0:128], identf[:], ALU.add, r=[("pp", h, 0), "identf"], w=[("xx", h)])
                  if n == int(_os.environ.get('GDN_NMIN', '0')):
                      cut(5.1)
                  for h in hs:
                      kB = ("ps", 2 + h)
                      B_ = PS[2 + h]
                      tr(B_[:, 128:256], PP[h][0][:, 0:128], identf[:], r=[("pp", h, 0), "identf", kB], w=[kB])
                      act(PP[h][0][:, 128:256], B_[:, 128:256], AF.Copy, r=[kB], w=[("pp", h, 0)])
                  if n == int(_os.environ.get('GDN_NMIN', '0')):
                      cut(5.2)
                  for lvl in range(1, 7):
                      src = (lvl - 1) % 2
                      dstk = lvl % 2
                      for h in hs:
                          kB = ("ps", 2 + h)
                          B_ = PS[2 + h]
                          Ps = PP[h][src]
                          Pd = PP[h][dstk]
                          if lvl < 6:
                              mm(B_[:, 0:128], Ps[:, 128:256], Ps[:, 0:128], r=[("pp", h, src), kB], w=[kB])
                          mm(B_[:, 128:256], Ps[:, 0:128], Ps[:, 128:256], r=[("pp", h, src), kB], w=[kB])
                          if lvl < 6:
                              act(Pd, B_[:, 0:256], AF.Copy, r=[kB], w=[("pp", h, dstk)])
                          else:
                              act(Pd[:, 128:256], B_[:, 128:256], AF.Copy, r=[kB], w=[("pp", h, dstk)])
                          mm(B_[:, 256:384], Pd[:, 128:256], XX[h], r=[("pp", h, dstk), ("xx", h), kB], w=[kB])
                          tt("dve", XX[h], XX[h], B_[:, 256:384], ALU.add, r=[kB, ("xx", h)], w=[("xx", h)])
                  if n == int(_os.environ.get('GDN_NMIN', '0')):
                      cut(5.3)
                  for h in hs:
                      idx = n * 4 + h
                      kA = ("ps", n % 2)
                      A_ = PS[n % 2]
                      kB = ("ps", 2 + h)
                      B_ = PS[2 + h]
                      Bbf = B_[:].bitcast(BF16)
                      kTc = GK[:, h, c0:c0 + 128]
                      vTc = GV[:, h, c0:c0 + 128]
                      act(XB[h], XX[h], AF.Copy, r=[("xx", h)], w=[("xb", h)])
                      mm(B_[:, 0:128], kTc, ident[:], r=[("big", 1, h), "ident", kB], w=[kB])
                      mm(B_[:, 128:256], vTc, ident[:], r=[("big", 2, h), "ident", kB], w=[kB])
                      ts("dve", KBG[h], B_[:, 0:128], COL["ebg"][:, idx:idx + 1], ALU.mult, r=[kB, "c_ebg"], w=[("kbg", h)])
                      ts("dve", KDEC[h], B_[:, 0:128], COL["edec"][:, idx:idx + 1], ALU.mult, r=[kB, "c_edec"], w=[("kdec", h)])
                      ts("dve", VBt[h], B_[:, 128:256], COL["beta"][:, idx:idx + 1], ALU.mult, r=[kB, "c_beta"], w=[("vb", h)])
                      mm(A_[:, h * 128:(h + 1) * 128], KBG[h], XB[h], r=[("kbg", h), ("xb", h)], w=[kA])
                      act(WTN[h], A_[:, h * 128:(h + 1) * 128], AF.Copy, r=[kA], w=[("wtn", h)], scale=-1.0)
                  if n == int(_os.environ.get('GDN_NMIN', '0')):
                      cut(5.4)
                  for h in hs:
                      idx = n * 4 + h
                      ci = 6 + (idx % 2)
                      kC = ("ps", ci)
                      C_ = PS[ci]
                      mm(C_[:, 0:128], XB[h], VBt[h], start=True, stop=(n == 0), r=[("xb", h), ("vb", h), kC], w=[kC])
                      if n > 0:
                          mm(C_[:, 0:128], WTN[h], Sbf[h][:], start=False, stop=True, r=[("wtn", h), ("sbf", h), kC], w=[kC])
                      act(VN[h], C_[:, 0:128], AF.Copy, r=[kC], w=[("vn", h)])
                      if n > 0:
                          mm(C_[:, 128:256], Sbf[h][:], QG[h], start=True, stop=False, r=[("sbf", h), ("qg", h), kC], w=[kC])
                      mm(C_[:, 128:256], VN[h], INTRA[h], start=(n == 0), stop=True, r=[("vn", h), ("intra", h), kC], w=[kC])
                      mm(C_[:, 256:384], KDEC[h], VN[h], r=[("kdec", h), ("vn", h), kC], w=[kC])
                      if n == 0:
                          cp("dve", Sst[h][:], C_[:, 256:384], r=[kC], w=[("sst", h)])
                      else:
                          stt("dve", Sst[h][:], Sst[h][:], COL["eglast"][:, idx:idx + 1], C_[:, 256:384], ALU.mult, ALU.add,
                              r=[kC, ("sst", h), "c_eglast"], w=[("sst", h)])
                      act(Sbf[h][:], Sst[h][:], AF.Copy, r=[("sst", h)], w=[("sbf", h)])
                      act(OSQ[h], C_[:, 128:256], AF.Square, r=[kC], w=[("osq", h)])
                      cp("dve", OTC[h], C_[:, 128:256], r=[kC], w=[("otc", h)])
                      mm(C_[:, 384:512], onesb[:], OSQ[h], r=["onesb", ("osq", h), kC], w=[kC])
                      act(R12[h], C_[:, 384:512], AF.Ln, r=[kC], w=[("r12", h)], scale=1.0 / 128, bias=EPS)
                      act(R12[h], R12[h], AF.Exp, r=[("r12", h)], w=[("r12", h)], scale=-0.5)
                      stt("dve", OG1[h], OTC[h], gnw[:, 0:1], R12[h], ALU.mult, ALU.mult,
                          r=[("otc", h), "gnw", ("r12", h)], w=[("og1", h)])
                      tt("dve", OGT[:, h, c0:c0 + 128], OG1[h], GG[:, h, c0:c0 + 128], ALU.mult,
                         r=[("og1", h), ("big", 3, h)], w=[("ogt", h)])
                  if n == int(_os.environ.get('GDN_NMIN', '0')):
                      cut(5.5)
              S.barrier()

              if dbg and l == 0:
                  for h in range(4):
                      cp("dve", ACC, OGT[:, h, :], r=[("ogt", h)], w=["acc"])
                      S.dma("sp", dbg_d["d_ogt"][h * 128:(h + 1) * 128, :], ACC, reads=["acc"], is_output=True)
                  S.barrier()

              cut(6)
              S.dma("sp", ppwb, postw_d[l].partition_broadcast(128), writes=["ppwb"])
              MT = [BIG[0], BIG[1]]
              for ps_ in range(2):
                  wm0 = wget()
                  wbr_i = wget(keep=1)
                  wm1 = wget(keep=2)
                  wms = [wm0, wm1]
                  wbr = WB[wbr_i][:].rearrange("p (k c) -> p k c", k=4)
                  src_o = OSB if ps_ == 0 else OGT
                  onm = "osb" if ps_ == 0 else "ogt"
                  for f in range(8):
                      wi = wms[f // 4]
                      wv = WB[wi][:].rearrange("p (k c) -> p k c", k=8)
                      fl = f % 4
                      for tg in range(4):
                          p1 = nextps()
                          for kc in range(8):
                              mm(PS[p1][:], wv[:, kc, fl * 128:(fl + 1) * 128], hT[:, kc, tg * 512:(tg + 1) * 512],
                                 start=(kc == 0), stop=(kc == 7), r=[("wb", wi), "hT"], w=[("ps", p1)])
                          tb = tg % 2
                          act(T512[tb], PS[p1][:], AF.Sigmoid, r=[("ps", p1)], w=[("t512", tb)])
                          p2 = nextps()
                          for kc in range(4):
                              mm(PS[p2][:], wbr[:, kc, f * 128:(f + 1) * 128], src_o[:, kc, tg * 512:(tg + 1) * 512],
                                 start=(kc == 0), stop=(kc == 3), r=[("wb", wbr_i), (onm, kc)], w=[("ps", p2)])
                          dstm = MT[f // 4][:, fl, tg * 512:(tg + 1) * 512]
                          mkey = ("big", f // 4, fl)
                          if ps_ == 0:
                              tt("dve", dstm, PS[p2][:], T512[tb], ALU.mult, r=[("ps", p2), ("t512", tb)], w=[mkey])
                          else:
                              tt("dve", T512[tb], PS[p2][:], T512[tb], ALU.mult, r=[("ps", p2), ("t512", tb)], w=[("t512", tb)])
                              tt("dve", dstm, T512[tb], dstm, ALU.add, r=[("t512", tb), mkey], w=[mkey])

              if dbg and l == 0:
                  S.barrier()
                  for f in range(8):
                      cp("dve", af(4608, 4608 + T), MT[f // 4][:, f % 4, :], r=[("big", f // 4, f % 4)], w=["dbgm"])
                      S.dma("sp", dbg_d["d_mT"][f * 128:(f + 1) * 128, :], af(4608, 4608 + T), reads=["dbgm"], is_output=True)
                  S.barrier()
                  S.dma("sp", ppwb, postw_d[l].partition_broadcast(128), writes=["ppwb"])

              cut(7)
              wo = [wget(), wget(keep=1)]
              for t_ in range(16):
                  b = t_ % 2
                  pys = []
                  for half in range(2):
                      pi = nextps()
                      wv = WB[wo[half]][:].rearrange("p (k c) -> p k c", k=8)
                      for kc in range(8):
                          mm(PS[pi][:], MT[kc // 4][:, kc % 4, t_ * 128:(t_ + 1) * 128], wv[:, kc, :], start=(kc == 0), stop=(kc == 7),
                             r=[("big", kc // 4, kc % 4), ("wb", wo[half])], w=[("ps", pi)])
                      pys.append(pi)
                      act(junk[:, 0:512], PS[pi][:], AF.Square, r=[("ps", pi)], w=["junk", ("ssy", half)], accum=ssc[:, 6 + half:7 + half])
                  S.dma("sp", xt[b], xin[t_ * 128:(t_ + 1) * 128, :], reads=[xkey(t_)], writes=[("xt", b)])
                  tt("dve", ssc[:, b:b + 1], ssc[:, 6:7], ssc[:, 7:8], ALU.add, r=[("ssy", 0), ("ssy", 1)], w=[("ssc", b)])
                  act(ssc[:, 2 + b:3 + b], ssc[:, b:b + 1], AF.Ln, r=[("ssc", b)], w=[("ssc2", b)], scale=1.0 / D, bias=EPS)
                  act(ssc[:, 4 + b:5 + b], ssc[:, 2 + b:3 + b], AF.Exp, r=[("ssc2", b)], w=[("ssc4", b)], scale=-0.5)
                  for half in range(2):
                      hs_ = slice(half * 512, (half + 1) * 512)
                      stt("dve", T512[half], PS[pys[half]][:], ssc[:, 4 + b:5 + b], ppwb[:, hs_], ALU.mult, ALU.mult,
                          r=[("ps", pys[half]), ("ssc4", b), "ppwb"], w=[("t512", half)])
                      tt("dve", xt[b][:, hs_], xt[b][:, hs_], T512[half], ALU.add, r=[("xt", b), ("t512", half)], w=[("xt", b)])
                  S.dma("sp", out_d[t_ * 128:(t_ + 1) * 128, :], xt[b], reads=[("xt", b)], writes=[("out", t_)], is_output=True)
          except _Stop:
            break

        S.finish()
        S.emit()
    return nc


_PROG = {}


def _get_prog(NL, part):
    k = (NL, part)
    if k not in _PROG:
        _PROG[k] = build_program(NL, part=part)
    return _PROG[k]


def _host_layout(inputs, layers):
    f = lambda a: np.ascontiguousarray(np.asarray(a, dtype=np.float32))
    ls = list(layers)
    cw = np.asarray(inputs["conv_w"], dtype=np.float32)[ls]
    cw = cw.reshape(len(ls), 4, 12, 128).transpose(0, 3, 2, 1).reshape(len(ls), 128, 48)
    d = {
        "pre_w": f(np.asarray(inputs["pre_norm_w"])[ls]),
        "post_w": f(np.asarray(inputs["post_norm_w"])[ls]),
        "w_in": f(np.asarray(inputs["w_in"])[ls]),
        "conv_w": f(cw),
        "a_log": f(np.tile(np.asarray(inputs["a_log"])[ls], (1, 16))),
        "dt_bias": f(np.tile(np.asarray(inputs["dt_bias"])[ls], (1, 16))),
        "gdn_norm_w": f(np.asarray(inputs["gdn_norm_w"])[ls]),
        "w_branch_sb": f(np.asarray(inputs["w_branch_sb"])[ls]),
        "w_branch_gdn": f(np.asarray(inputs["w_branch_gdn"])[ls]),
        "w_out": f(np.asarray(inputs["w_out"])[ls]),
    }
    return d


def kernel(**inputs):
    x = np.asarray(inputs["x"], dtype=np.float32)
    NLF = 2
    nc = _get_prog(NLF, 0)
    cur = [np.ascontiguousarray(x[b]) for b in range(NCORES)]
    for l0 in range(0, DEPTH, NLF):
        w = _host_layout(inputs, range(l0, l0 + NLF))
        in_maps = [dict(w, x=cur[b]) for b in range(NCORES)]
        res = run_bass_kernel_spmd(nc, in_maps, core_ids=list(range(NCORES)))
        cur = [np.ascontiguousarray(res.results[b]["out"]) for b in range(NCORES)]
    return np.stack(cur, axis=0).astype(np.float32)
```
